# Optimizing a Trainium2 kernel written in Bass

```python
import jax, jax.numpy as jnp
from jax import lax
import numpy as np

D_MODEL = 4096
BATCH = 2
SEQ = 4096
DEPTH = 2

GRID_W = 64
CTX_LEN = 256
NA_HEADS = 16
NA_HEAD_DIM = 128
NA_WIDTH = NA_HEADS * NA_HEAD_DIM
WIN_H = 8
WIN_W = 16
GMLP_GROUPS = 16
GMLP_WIDTH = 2048
GMLP_GROUP_DIM = GMLP_WIDTH // GMLP_GROUPS
CHUNK = 128
N_EXPERTS = 16
EXPERT_FF = D_MODEL // 4
CAPACITY_FACTOR = 2
N_MOD = 6
EPS = 1e-6
OFF_Q = 0
OFF_K = OFF_Q + NA_WIDTH
OFF_V = OFF_K + NA_WIDTH
OFF_U = OFF_V + NA_WIDTH
OFF_GV = OFF_U + GMLP_WIDTH
OFF_GA = OFF_GV + GMLP_WIDTH
OFF_GB = OFF_GA + D_MODEL
IN_COLS = OFF_GB + D_MODEL

kernel_name = "hybrid_natten_gmlp_ec_dit_block"


def rms_norm(x, g):
    xf = x.astype(jnp.float32)
    y = xf * lax.rsqrt(jnp.mean(xf * xf, axis=-1, keepdims=True) + EPS)
    return (y * g.astype(jnp.float32)).astype(x.dtype)


def layer_norm(x, g):
    xf = x.astype(jnp.float32)
    mu = jnp.mean(xf, axis=-1, keepdims=True)
    xc = xf - mu
    y = xc * lax.rsqrt(jnp.mean(xc * xc, axis=-1, keepdims=True) + EPS)
    return (y * g.astype(jnp.float32)).astype(x.dtype)


def modulate(h, shift, scale):
    return h * (1 + scale) + shift


def split_heads(t):
    return t.reshape(t.shape[:-1] + (NA_HEADS, NA_HEAD_DIM))


def neighbourhood_attention(q, k, v, k_ctx, v_ctx, rpb):
    B, N, H, dh = q.shape
    rows = N // GRID_W
    kh = min(WIN_H, rows)
    nloc = kh * WIN_W
    q = q * (dh ** -0.5)
    qg = q.reshape(B, rows, GRID_W, H, dh)
    kg = k.reshape(B, rows, GRID_W, H, dh)
    vg = v.reshape(B, rows, GRID_W, H, dh)
    cols = jnp.arange(GRID_W)
    col_start = jnp.clip(cols - WIN_W // 2, 0, GRID_W - WIN_W)
    col_idx = col_start[:, None] + jnp.arange(WIN_W)[None, :]
    col_off = col_idx - cols[:, None] + (WIN_W - 1)
    rpb_cols = rpb[:, :, col_off]

    def row_block(r):
        rs = jnp.clip(r - kh // 2, 0, rows - kh)
        q_r = lax.dynamic_index_in_dim(qg, r, axis=1, keepdims=False)
        k_band = lax.dynamic_slice_in_dim(kg, rs, kh, axis=1)
        v_band = lax.dynamic_slice_in_dim(vg, rs, kh, axis=1)
        k_win = k_band[:, :, col_idx]
        v_win = v_band[:, :, col_idx]
        row_off = rs + jnp.arange(kh) - r + (WIN_H - 1)
        bias = jnp.transpose(rpb_cols[:, row_off], (0, 2, 1, 3))
        s_loc = jnp.einsum('bqhd,biqjhd->bhqij', q_r, k_win).astype(jnp.float32) + bias.astype(jnp.float32)
        s_ctx = jnp.einsum('bqhd,bchd->bhqc', q_r, k_ctx).astype(jnp.float32)
        s = jnp.concatenate([s_loc.reshape(B, H, GRID_W, nloc), s_ctx], axis=-1)
        p = jax.nn.softmax(s, axis=-1).astype(v.dtype)
        p_loc = p[..., :nloc].reshape(B, H, GRID_W, kh, WIN_W)
        p_ctx = p[..., nloc:]
        return (jnp.einsum('bhqij,biqjhd->bqhd', p_loc, v_win)
                + jnp.einsum('bhqc,bchd->bqhd', p_ctx, v_ctx))

    o = lax.map(row_block, jnp.arange(rows))
    return jnp.moveaxis(o, 0, 1).reshape(B, N, H * dh)


def context_attention(q, k, v):
    B, Nc, H, dh = q.shape
    s = jnp.einsum('bqhd,bkhd->bhqk', q * (dh ** -0.5), k).astype(jnp.float32)
    p = jax.nn.softmax(s, axis=-1).astype(v.dtype)
    return jnp.einsum('bhqk,bkhd->bqhd', p, v).reshape(B, Nc, H * dh)


def spatial_gating(u, v, norm_g, ws, bs):
    B, N, _ = u.shape
    u = jax.nn.gelu(u)
    v = layer_norm(jax.nn.gelu(v), norm_g)
    vc = v.reshape(B, N // CHUNK, CHUNK, GMLP_GROUPS, GMLP_GROUP_DIM)
    mixed = jnp.einsum('gpq,bnqgc->bnpgc', ws, vc) + jnp.transpose(bs)[:, :, None]
    return u * mixed.reshape(B, N, GMLP_WIDTH)


def gated_merge(o_a, o_b, gate_a, gate_b, w_branch_a, w_branch_b, w_out):
    y = jax.nn.sigmoid(gate_a) * (o_a @ w_branch_a) + jax.nn.sigmoid(gate_b) * (o_b @ w_branch_b)
    return y @ w_out


def hybrid_mixer(h, h_ctx, w_in, rpb, gmlp_norm_g, gmlp_ws, gmlp_bs, w_branch_a, w_branch_b, w_out,
                 with_ctx_out):
    p = h @ w_in
    if with_ctx_out:
        pc = h_ctx @ w_in
        k_ctx = split_heads(pc[..., OFF_K:OFF_V])
        v_ctx = split_heads(pc[..., OFF_V:OFF_U])
    else:
        kv = h_ctx @ w_in[:, OFF_K:OFF_U]
        k_ctx = split_heads(kv[..., :NA_WIDTH])
        v_ctx = split_heads(kv[..., NA_WIDTH:])
    o_a = neighbourhood_attention(split_heads(p[..., OFF_Q:OFF_K]), split_heads(p[..., OFF_K:OFF_V]),
                                  split_heads(p[..., OFF_V:OFF_U]), k_ctx, v_ctx, rpb)
    o_b = spatial_gating(p[..., OFF_U:OFF_GV], p[..., OFF_GV:OFF_GA], gmlp_norm_g, gmlp_ws, gmlp_bs)
    y_lat = gated_merge(o_a, o_b, p[..., OFF_GA:OFF_GB], p[..., OFF_GB:IN_COLS],
                        w_branch_a, w_branch_b, w_out)
    if not with_ctx_out:
        return y_lat, None
    oc_a = context_attention(split_heads(pc[..., OFF_Q:OFF_K]), k_ctx, v_ctx)
    oc_b = spatial_gating(pc[..., OFF_U:OFF_GV], pc[..., OFF_GV:OFF_GA], gmlp_norm_g, gmlp_ws, gmlp_bs)
    y_ctx = gated_merge(oc_a, oc_b, pc[..., OFF_GA:OFF_GB], pc[..., OFF_GB:IN_COLS],
                        w_branch_a, w_branch_b, w_out)
    return y_lat, y_ctx


def expert_choice_ffn(h, router_w, w1, w3, w2):
    B, N, _ = h.shape
    cap = CAPACITY_FACTOR * N // N_EXPERTS
    aff = jax.nn.softmax((h @ router_w).astype(jnp.float32), axis=-1)
    g, idx = lax.top_k(jnp.transpose(aff, (0, 2, 1)), cap)
    bidx = jnp.arange(B)[:, None, None]
    xs = h[bidx, idx]
    hid = jax.nn.silu(jnp.einsum('becd,edf->becf', xs, w1)) * jnp.einsum('becd,edf->becf', xs, w3)
    out = jnp.einsum('becf,efd->becd', hid, w2) * g.astype(h.dtype)[..., None]
    return jnp.zeros_like(h).at[bidx, idx].add(out)


def setup_inputs(seed: int = 0) -> dict:
    key = jax.random.key(seed)
    ks = jax.random.split(key, 24)
    f32 = jnp.float32

    def nrm(k, shape, scale):
        return jax.random.normal(k, shape, f32) * scale

    D = D_MODEL
    return {
        "x": nrm(ks[0], (BATCH, SEQ, D), 1.0),
        "c": nrm(ks[1], (BATCH, D), 1.0),
        "ctx": nrm(ks[2], (BATCH, CTX_LEN, D), 1.0),
        "c_ctx": nrm(ks[3], (D,), 1.0),
        "ada_w": nrm(ks[4], (DEPTH, D, N_MOD * D), 0.3 * D ** -0.5),
        "ada_b": nrm(ks[5], (DEPTH, N_MOD * D), 0.02),
        "norm1_g": 1.0 + nrm(ks[6], (DEPTH, D), 0.02),
        "norm2_g": 1.0 + nrm(ks[7], (DEPTH, D), 0.02),
        "w_in": nrm(ks[8], (DEPTH, D, IN_COLS), D ** -0.5),
        "na_rpb": nrm(ks[9], (DEPTH, NA_HEADS, 2 * WIN_H - 1, 2 * WIN_W - 1), 0.1),
        "gmlp_norm_g": 1.0 + nrm(ks[10], (DEPTH, GMLP_WIDTH), 0.02),
        "gmlp_ws": nrm(ks[11], (DEPTH, GMLP_GROUPS, CHUNK, CHUNK), CHUNK ** -0.5),
        "gmlp_bs": 1.0 + nrm(ks[12], (DEPTH, GMLP_GROUPS, CHUNK), 0.02),
        "w_branch_a": nrm(ks[13], (DEPTH, NA_WIDTH, D), NA_WIDTH ** -0.5),
        "w_branch_b": nrm(ks[14], (DEPTH, GMLP_WIDTH, D), GMLP_WIDTH ** -0.5),
        "w_out": nrm(ks[15], (DEPTH, D, D), D ** -0.5),
        "router_w": nrm(ks[16], (DEPTH, D, N_EXPERTS), D ** -0.5),
        "exp_w1": nrm(ks[17], (DEPTH, N_EXPERTS, D, EXPERT_FF), D ** -0.5),
        "exp_w3": nrm(ks[18], (DEPTH, N_EXPERTS, D, EXPERT_FF), D ** -0.5),
        "exp_w2": nrm(ks[19], (DEPTH, N_EXPERTS, EXPERT_FF, D), EXPERT_FF ** -0.5),
        "final_norm_g": 1.0 + nrm(ks[20], (D,), 0.02),
    }


def reference(x, c, ctx, c_ctx, ada_w, ada_b, norm1_g, norm2_g, w_in, na_rpb, gmlp_norm_g, gmlp_ws,
              gmlp_bs, w_branch_a, w_branch_b, w_out, router_w, exp_w1, exp_w3, exp_w2, final_norm_g):
    D = D_MODEL
    x_lat, x_ctx = x, ctx
    silu_c = jax.nn.silu(c)
    silu_cc = jax.nn.silu(c_ctx)
    for layer in range(DEPTH):
        last = layer == DEPTH - 1
        mod = (silu_c @ ada_w[layer] + ada_b[layer])[:, None, :]
        sh1, sc1, gt1, sh2, sc2, gt2 = jnp.split(mod, N_MOD, axis=-1)
        n_ctx_mod = 2 if last else N_MOD
        mod_c = silu_cc @ ada_w[layer][:, :n_ctx_mod * D] + ada_b[layer][:n_ctx_mod * D]
        mod_c = jnp.split(mod_c, n_ctx_mod, axis=-1)

        h = modulate(rms_norm(x_lat, norm1_g[layer]), sh1, sc1)
        h_ctx = modulate(rms_norm(x_ctx, norm1_g[layer]), mod_c[0], mod_c[1])
        y_lat, y_ctx = hybrid_mixer(h, h_ctx, w_in[layer], na_rpb[layer], gmlp_norm_g[layer],
                                    gmlp_ws[layer], gmlp_bs[layer], w_branch_a[layer],
                                    w_branch_b[layer], w_out[layer], not last)
        x_lat = x_lat + gt1 * y_lat

        h = modulate(rms_norm(x_lat, norm2_g[layer]), sh2, sc2)
        x_lat = x_lat + gt2 * expert_choice_ffn(h, router_w[layer], exp_w1[layer], exp_w3[layer],
                                                exp_w2[layer])
        if not last:
            x_ctx = x_ctx + mod_c[2] * y_ctx
            h_ctx = modulate(rms_norm(x_ctx, norm2_g[layer]), mod_c[3], mod_c[4])
            x_ctx = x_ctx + mod_c[5] * expert_choice_ffn(h_ctx, router_w[layer], exp_w1[layer],
                                                         exp_w3[layer], exp_w2[layer])
    return rms_norm(x_lat, final_norm_g)
```

```python
import numpy as np
import concourse.bass as bass
import concourse.mybir as mybir
from concourse.bass_utils import run_bass_kernel_spmd

F32 = mybir.dt.float32
BF16 = mybir.dt.bfloat16
AF = mybir.ActivationFunctionType
ALU = mybir.AluOpType
AX = mybir.AxisListType

FULL = dict(D=4096, N=4096, C=256, H=16, GW=2048, E=16, FF=1024, L=2)
NEG = -30000.0
EPS = 1e-6
NB = 512
NITER = 30


class Sched:
    def __init__(self):
        self.ops = []
        self.lastw = {}
        self.readers = {}

    def op(self, eng, fn, r=(), w=(), sig=True, dma=False):
        deps = set()
        for k in r:
            if k in self.lastw:
                deps.add(self.lastw[k])
        for k in w:
            if k in self.lastw:
                deps.add(self.lastw[k])
            deps.update(self.readers.get(k, ()))
        idx = len(self.ops)
        self.ops.append([eng, fn, deps, sig or dma, dma])
        for k in w:
            self.lastw[k] = idx
            self.readers[k] = set()
        for k in r:
            if k not in w:
                self.readers.setdefault(k, set()).add(idx)
        return idx

    def emit(self, nc, es):
        engs = ['pe', 'act', 'dve', 'pool', 'sp']
        KD_ = 8
        csem = {e: es.enter_context(nc.semaphore('c_' + e)) for e in engs}
        dsem = {e: [es.enter_context(nc.semaphore('d_%s%d' % (e, i))) for i in range(KD_)] for e in ('pool', 'sp', 'act')}
        cnt = {e: 0 for e in engs}
        dcnt = {e: [0] * KD_ for e in dsem}
        drr = {e: 0 for e in dsem}
        sigof = [None] * len(self.ops)
        pre = [None] * len(self.ops)
        pend = {e: [] for e in engs}
        for i, (eng, fn, deps, sig, dma) in enumerate(self.ops):
            if dma:
                k = drr[eng] % KD_
                drr[eng] += 1
                if dcnt[eng][k] > 0:
                    pre[i] = (dsem[eng][k], dcnt[eng][k])
                dcnt[eng][k] += 16
                sigof[i] = (dsem[eng][k], dcnt[eng][k], 16)
            elif sig:
                cnt[eng] += 1
                sigof[i] = (csem[eng], cnt[eng], 1)
                for j in pend[eng]:
                    sigof[j] = (csem[eng], cnt[eng], 0)
                pend[eng] = []
            else:
                pend[eng].append(i)
        for e in engs:
            assert not pend[e], 'trailing unsignalled ops on ' + e
        per = {e: [] for e in engs}
        waited = {e: {} for e in engs}
        for i, (eng, fn, deps, sig, dma) in enumerate(self.ops):
            need = {}
            if pre[i] is not None:
                need[id(pre[i][0])] = pre[i]
            for d in deps:
                de = self.ops[d]
                if de[0] == 'pe' and eng == 'pe' and not de[4] and not dma:
                    continue
                s, v, _ = sigof[d]
                if id(s) not in need or need[id(s)][1] < v:
                    need[id(s)] = (s, v)
            ws = []
            for sid, (s, v) in need.items():
                if waited[eng].get(sid, 0) >= v:
                    continue
                waited[eng][sid] = v
                ws.append((s, v))
            so = sigof[i] if (sigof[i] is not None and sigof[i][2] > 0) else None
            per[eng].append((ws, fn, so))
        self.ops = None
        return per


def build(cfg):
    D, N, C, H, GW, E, FF, L = (cfg[k] for k in ('D', 'N', 'C', 'H', 'GW', 'E', 'FF', 'L'))
    KD, T, NA, G, KF = D // 128, N + C, H * 128, GW // 128, FF // 128
    KA = NA // 128
    rows = N // 64
    cap, capc = 2 * N // E, 2 * C // E
    NCOL = (3 * NA + 2 * GW + 2 * D) // 128
    jQ, jK, jV, jU, jGV, jGA, jGB = 0, KA, 2 * KA, 3 * KA, 3 * KA + G, 3 * KA + 2 * G, 3 * KA + 2 * G + KD
    blocks = [(t0, NB) for t0 in range(0, N, NB)] + [(N, C)]
    WMAX = max(KD, KA, G, KF) * 128 * 2
    assert WMAX <= 8192

    nc = bass.Bass("TRN2", target_bir_lowering=False)

    def din(name, shape):
        return nc.dram_tensor(name, list(shape), F32, kind="ExternalInput").ap()

    xT = din("xT", [D, T]); cT = din("cT", [128, KD * 2])
    adaw = din("adaw", [L, 6 * KD, 128, KD * 128]); adab = din("adab", [L, 128, 6 * KD])
    g1 = din("g1", [L, 128, KD]); g2 = din("g2", [L, 128, KD]); gf = din("gf", [128, KD])
    win = din("win", [L, NCOL, 128, KD * 128]); biasd = din("bias", [L, H, 128, 5 * 640])
    gng = din("gng", [L, 1, GW]); wsT = din("wsT", [L, 128, G * 128]); gbs = din("gbs", [L, 1, G * 128])
    wa = din("wa", [L, KD, 128, KA * 128]); wb = din("wb", [L, KD, 128, G * 128]); wo = din("wo", [L, KD, 128, KD * 128])
    rw = din("rw", [L, 128, KD * E])
    w1 = din("w1", [L, E, KF, 128, KD * 128]); w3 = din("w3", [L, E, KF, 128, KD * 128])
    w2 = din("w2", [L, E, KD, 128, KF * 128]); seld = din("sel", [64, E * 128]); identd = din("ident", [128, 128])
    yT = nc.dram_tensor("yT", [D, N], F32, kind="ExternalOutput").ap()

    def dscr(name, shape, dt):
        return nc.dram_tensor(name, list(shape), dt).ap()

    xs = dscr("xs", [D, T], F32)
    QT = dscr("QT", [NA, T], BF16); KT = dscr("KT", [NA, T], BF16); VT = dscr("VT", [NA, T], BF16)
    OAT = dscr("OAT", [NA, T], BF16); OBT = dscr("OBT", [GW, T], BF16)
    SGA = dscr("SGA", [D, T], BF16); SGB = dscr("SGB", [D, T], BF16); H2T = dscr("H2T", [D, T], BF16)
    AFFT = dscr("AFFT", [E, T], F32); GMT = dscr("GMT", [E, T], F32)

    import contextlib
    es = contextlib.ExitStack()
    ARENA = 172 * 1024
    arena = es.enter_context(nc.sbuf_tensor("arena", [128, ARENA // 4], F32))
    off = [0]

    def reg(nbytes):
        o = off[0]
        off[0] += (nbytes + 63) // 64 * 64
        assert off[0] <= ARENA, off[0]
        return o

    def view(o, nbytes, dt, pat=None, p0=0, p1=128, **kw):
        v = arena[p0:p1, o // 4:(o + nbytes) // 4]
        if dt == BF16:
            v = v.bitcast(BF16)
        if pat:
            v = v.rearrange(pat, **kw)
        return v

    oXS = reg(32 * 1024); oH = reg(32 * 1024); oW = reg(3 * 8192); oBIG = reg(64 * 1024); oM = reg(12 * 1024); oCA = reg(8 * 1024)
    Hv = view(oH, KD * NB * 2, BF16, "p (c n) -> p c n", c=KD)
    Wsl = [oW + i * 8192 for i in range(3)]
    o = oCA
    MODv = []
    for l in range(L):
        MODv.append(view(o, 6 * KD * 2 * 4, F32, "p (j t) -> p j t", t=2)); o += 6 * KD * 8
    A1 = view(o, KD * 8, F32, "p (c t) -> p c t", t=2); o += KD * 8
    A2 = view(o, KD * 8, F32, "p (c t) -> p c t", t=2); o += KD * 8
    G1v = view(o, KD * 4, F32); o += KD * 4
    G2v = view(o, KD * 4, F32); o += KD * 4
    GFv = view(o, KD * 4, F32); o += KD * 4
    ONES = view(o, 256, BF16); o += 256
    IDB = view(o, 256, BF16); o += 256
    IDF = view(o, 512, F32); o += 512
    RWv = view(o, KD * E * 2, BF16, "p (c e) -> p c e", e=E); o += KD * E * 2
    SCT = view(o, KD * 2 * 2, BF16, "p (c t) -> p c t", t=2); o += KD * 4
    CTf = view(o, KD * 2 * 4, F32); o += KD * 8
    ADB = view(o, 6 * KD * 4, F32); o += 6 * KD * 4
    SM = view(o, 64 * 4, F32); o += 256
    assert o <= oCA + 8 * 1024, o - oCA

    ps = [es.enter_context(nc.psum_tensor("ps%d" % i, [128, 512], F32)) for i in range(7)]
    S = Sched()
    bank = [0]

    def nb():
        bank[0] = (bank[0] + 1) % 5
        return bank[0]

    wrr = [0]

    def wload(src, nbytes_bf, pat, **kw):
        k = wrr[0] % 3
        wrr[0] += 1
        v = view(Wsl[k], nbytes_bf, BF16)
        S.op('pool', lambda e: e.dma_start(out=v, in_=src, max_dma_last_dim=8192), r=(), w=(('W', k),), dma=True)
        return k, v.rearrange(pat, **kw)

    def mm(out, lhsT, rhs, st, sp, r, w, sig):
        S.op('pe', lambda e: e.matmul(out, lhsT, rhs, start=st, stop=sp), r=r, w=w, sig=sig)

    def act(out, in_, func, r, w, bias=None, scale=None, accum=None):
        kw = {}
        if bias is not None:
            kw['bias'] = bias
        if scale is not None:
            kw['scale'] = scale
        if accum is not None:
            kw['accum_out'] = accum
        S.op('act', lambda e: e.activation(out, in_, func, **kw), r=r, w=w)

    def dve(fn, r, w):
        S.op('dve', fn, r=r, w=w)

    def dma(out, in_, r, w, q='sp'):
        S.op(q, lambda e: e.dma_start(out=out, in_=in_), r=r, w=w, dma=True)

    def fence(keys):
        S.op('dve', lambda e: e.memset(SM[:, 55:56], 0.0), r=(), w=tuple(keys))

    def gelu_tanh(out_ap, x_ps, shp, r, w, accum=None):
        t1 = TMPv[:, :shp]; t2 = SDv[:, :shp]
        act(t1, x_ps, AF.Square, r, ('TMP',))
        dve(lambda e: e.tensor_scalar(t1, t1, 0.044715, 1.0, ALU.mult, ALU.add), ('TMP',), ('TMP',))
        dve(lambda e: e.tensor_tensor(t1, t1, x_ps, ALU.mult), ('TMP',) + tuple(r), ('TMP',))
        act(t2, t1, AF.Sigmoid, ('TMP',), ('SD',), scale=1.5957691216)
        if accum is None:
            dve(lambda e: e.tensor_tensor(out_ap, t2, x_ps, ALU.mult), ('SD',) + tuple(r), w)
        else:
            dve(lambda e: e.scalar_tensor_tensor(out_ap, t2, 1.0, x_ps, ALU.mult, ALU.mult, accum_out=accum), ('SD',) + tuple(r), w)

    def chunked(ap):
        return ap.rearrange("(c p) t -> p c t", p=128)

    class _Stop(Exception):
        pass

    def ck(name):
        if cfg.get('stop') == name:
            raise _Stop()

    try:
        dma(CTf, cT, (), ('CT',)); dma(GFv, gf, (), ('GF',)); dma(IDF, identd, (), ('IDF',))
        dve(lambda e: e.memset(ONES, 1.0), (), ('ONES',))
        dve(lambda e: e.tensor_copy(IDB, IDF), ('IDF',), ('IDB',))
        act(SCT, CTf.rearrange("p (c t) -> p c t", t=2), AF.Silu, ('CT',), ('SCT',))

        def psum_evac_copy(i, out, in_, r, w):
            if i % 2 == 0:
                act(out, in_, AF.Copy, r, w)
            else:
                dve(lambda e: e.tensor_copy(out, in_), r, w)

        sm_i = [0]

        def smcol(p0=0, p1=128):
            sm_i[0] = (sm_i[0] + 1) % 55
            return SM[p0:p1, sm_i[0]:sm_i[0] + 1], ('SM', sm_i[0])

        def rms_block(get_x, n, Afn, Bfn, outfn, sqv, sdv, tmpv, sqk='SQ', sdk='SD'):
            nq = KD // 4
            for q in range(nq):
                xv, xk = get_x(q, 0)
                act(sqv[:, :, :n], xv, AF.Square, xk, (sqk,))
                for i in range(4):
                    mm(ps[6][:, :n], ONES, sqv[:, i, :n], q == 0 and i == 0, q == nq - 1 and i == 3, (sqk, 'ONES'), (('ps', 6),), i == 3)
            act(sdv[:, :n], ps[6][:, :n], AF.Sqrt, (('ps', 6), 'EPS'), (sdk,), bias=EPSv, scale=1.0 / D)
            dve(lambda e: e.reciprocal(sdv[:, :n], sdv[:, :n]), (sdk,), (sdk,))
            for q in range(nq):
                xv, xk = get_x(q, 1)
                for i in range(4):
                    c = q * 4 + i
                    a_ap, ak = Afn(c)
                    if Bfn is None:
                        o_ap, okk = outfn(c)
                        dve(lambda e, xv=xv, i=i, a_ap=a_ap, o_ap=o_ap: e.scalar_tensor_tensor(o_ap, xv[:, i, :], a_ap, sdv[:, :n], ALU.mult, ALU.mult),
                            tuple(xk) + (sdk,) + ak, okk)
                    else:
                        dve(lambda e, xv=xv, i=i, a_ap=a_ap: e.scalar_tensor_tensor(tmpv[:, :n], xv[:, i, :], a_ap, sdv[:, :n], ALU.mult, ALU.mult),
                            tuple(xk) + (sdk,) + ak, ('TMP',))
                        b_ap, bk = Bfn(c)
                        o_ap, okk = outfn(c)
                        act(o_ap, tmpv[:, :n], AF.Identity, ('TMP',) + bk, okk, bias=b_ap)

        EPSv = SM[:, 63:64]
        dve(lambda e: e.memset(EPSv, EPS), (), ('EPS',))

        SQv = view(oM, 4 * NB * 2, BF16, "p (c n) -> p c n", c=4)
        SDv = view(oM + 4096, NB * 4, F32)
        TMPv = view(oM + 6144, NB * 4, F32)
        OST = [view(oM + 8192 + i * 1024, NB * 2, BF16) for i in range(4)]
        ost_i = [0]

        def ostage():
            ost_i[0] = (ost_i[0] + 1) % 4
            return OST[ost_i[0]], ('OST', ost_i[0])

        ck('setup')
        for l in range(L):
            dma(ADB, adab[l], (), ('ADB',))
            for j in range(6 * KD):
                k, wv = wload(adaw[l, j], KD * 256, "p (c m) -> p c m", c=KD)
                b = nb()
                for c in range(KD):
                    mm(ps[b][:, 0:2], wv[:, c, :], SCT[:, c, :], c == 0, c == KD - 1, (('W', k), 'SCT'), (('ps', b),), c == KD - 1)
                dve(lambda e, b=b, j=j, l=l: e.tensor_scalar(MODv[l][:, j, :], ps[b][:, 0:2], ADB[:, j:j + 1], None, ALU.add),
                    (('ps', b), 'ADB'), (('MOD', l),))

        ck('mod')
        for l in range(L):
            last = l == L - 1
            MOD = MODv[l]
            mk = (('MOD', l),)
            dma(G1v, g1[l], (), ('G1',)); dma(G2v, g2[l], (), ('G2',))
            dma(RWv.rearrange("p c e -> p (c e)"), rw[l], (), ('RW',), q='pool')
            for t in range(2):
                dve(lambda e, t=t, MOD=MOD: e.tensor_scalar(A1[:, :, t], MOD[:, KD:2 * KD, t], 1.0, None, ALU.add), mk, ('A1',))
                dve(lambda e, t=t, MOD=MOD: e.tensor_tensor(A1[:, :, t], A1[:, :, t], G1v, ALU.mult), ('A1', 'G1'), ('A1',))
                dve(lambda e, t=t, MOD=MOD: e.tensor_scalar(A2[:, :, t], MOD[:, 4 * KD:5 * KD, t], 1.0, None, ALU.add), mk, ('A2',))
                dve(lambda e, t=t, MOD=MOD: e.tensor_tensor(A2[:, :, t], A2[:, :, t], G2v, ALU.mult), ('A2', 'G2'), ('A2',))
            xsrc = xT if l == 0 else xs

            GBUF = [view(oBIG + i * 2 * GW, 2 * GW, BF16) for i in range(4)]
            VTOK = [view(oBIG + 8 * GW + i * 2 * GW, 2 * GW, BF16) for i in range(4)]
            UTv = view(oBIG + 16 * GW, G * NB * 2, BF16, "p (g n) -> p g n", g=G)
            o2 = oBIG + 16 * GW + G * NB * 2
            WSv = view(o2, G * 256, BF16, "p (g m) -> p g m", g=G); o2 += G * 256
            BSB = view(o2, G * 512, F32, "p (g m) -> p g m", g=G); o2 += G * 512
            GNG = view(o2, GW * 2, BF16); o2 += GW * 2
            assert o2 <= oBIG + 64 * 1024
            GSUM = view(oXS + 16 * 1024, 4 * G * 4, F32, "p (t g) -> p t g", t=4)
            VTMP = view(oXS + 20 * 1024, GW * 4, F32)
            fence(('WS', 'BSB', 'GNG', 'UT', 'VTMP', 'SQ', 'SD', 'TMP') + tuple(('GBUF', i) for i in range(4)) + tuple(('VTOK', i) for i in range(4)) + tuple(('XS', i) for i in range(2)) + tuple(('GSUM', i) for i in range(4)) + tuple(('OST', i) for i in range(4)) + ('ACC', 'SEL', 'SA', 'TT', 'GB', 'GM', 'HID', 'SQ8', ('XP', 0), ('XP', 1)) + ('VV', 'JK') + ('XB', 'OAb', 'OBb', 'AF', 'EX', 'AFT'))
            dma(WSv.rearrange("p g m -> p (g m)"), wsT[l], (), ('WS',), q='pool')
            dma(BSB.rearrange("p g m -> p (g m)"), gbs[l].partition_broadcast(128), (), ('BSB',))
            dma(GNG, gng[l].partition_broadcast(128), (), ('GNG',), q='pool')
            XSb = [view(oXS + i * 8192, 4 * NB * 4, F32, "p (c n) -> p c n", c=4) for i in range(2)]
            xrr = [0]
            for (t0, n) in blocks:
                isctx = t0 >= N
                tc_ = 1 if isctx else 0
                nt4 = n // 128

                def get_x(q, pas, t0=t0, n=n):
                    k = xrr[0] % 2
                    xrr[0] += 1
                    dma(XSb[k][:, :, :n], chunked(xsrc)[:, q * 4:q * 4 + 4, t0:t0 + n], ('xs',), (('XS', k),))
                    return XSb[k][:, :, :n], (('XS', k),)
                rms_block(get_x, n, lambda c: (A1[:, c, tc_:tc_ + 1], ('A1',)), lambda c: (MOD[:, c, tc_:tc_ + 1], mk),
                          lambda c: (Hv[:, c, :n], ('H',)), SQv, SDv, TMPv)
                if isctx and last:
                    jlist = list(range(jK, jU))
                else:
                    jlist = list(range(NCOL))
                for j in jlist:
                    k, wv = wload(win[l, j], KD * 256, "p (c m) -> p c m", c=KD)
                    if jGV <= j < jGA:
                        jj = j - jGV
                        for t4 in range(nt4):
                            b = nb()
                            for c in range(KD):
                                mm(ps[b][:, :128], Hv[:, c, t4 * 128:(t4 + 1) * 128], wv[:, c, :], c == 0, c == KD - 1,
                                   (('W', k), 'H'), (('ps', b),), c == KD - 1)
                            gelu_tanh(GBUF[t4][:, jj * 128:(jj + 1) * 128], ps[b][:, :128], 128, (('ps', b),), (('GBUF', t4), ('GSUM', t4)),
                                      accum=GSUM[:, t4, jj:jj + 1])
                        if jj == G - 1:
                            for t4 in range(nt4):
                                s1, k1 = smcol(); s2, k2 = smcol(); mean, k3 = smcol(); var, k4 = smcol()
                                dve(lambda e, t4=t4, s1=s1: e.reduce_sum(s1, GSUM[:, t4, :], axis=AX.X), (('GSUM', t4),), (k1,))
                                act(VTOK[t4], GBUF[t4], AF.Square, (('GBUF', t4),), (('VTOK', t4), k2), accum=s2)
                                dve(lambda e, s1=s1, mean=mean: e.tensor_scalar(mean, s1, 1.0 / GW, None, ALU.mult), (k1,), (k3,))
                                dve(lambda e, mean=mean, var=var: e.tensor_tensor(var, mean, mean, ALU.mult), (k3,), (k4,))
                                dve(lambda e, s2=s2, var=var: e.scalar_tensor_tensor(var, s2, 1.0 / GW, var, ALU.mult, ALU.subtract), (k2, k4), (k4,))
                                act(var, var, AF.Sqrt, (k4, 'EPS'), (k4,), bias=EPSv)
                                dve(lambda e, var=var: e.reciprocal(var, var), (k4,), (k4,))
                                dve(lambda e, t4=t4, mean=mean: e.scalar_tensor_tensor(VTMP, GBUF[t4], mean, GNG, ALU.subtract, ALU.mult),
                                    (('GBUF', t4), k3, 'GNG'), ('VTMP',))
                                act(VTOK[t4], VTMP, AF.Identity, ('VTMP', k4), (('VTOK', t4),), scale=var)
                                for g in range(G):
                                    b = nb()
                                    mm(ps[b][:, :128], VTOK[t4][:, g * 128:(g + 1) * 128], WSv[:, g, :], True, True,
                                       (('VTOK', t4), 'WS'), (('ps', b),), True)
                                    dve(lambda e, b=b, g=g: e.tensor_tensor(TMPv[:, :128], ps[b][:, :128], BSB[:, g, :], ALU.add),
                                        (('ps', b), 'BSB'), ('TMP',))
                                    ov, ok_ = ostage()
                                    dve(lambda e, g=g, t4=t4, ov=ov: e.tensor_tensor(ov[:, :128], TMPv[:, :128], UTv[:, g, t4 * 128:(t4 + 1) * 128], ALU.mult),
                                        ('TMP', 'UT'), (ok_,))
                                    dma(OBT[g * 128:(g + 1) * 128, t0 + t4 * 128:t0 + (t4 + 1) * 128], ov[:, :128], (ok_,), ('OBT',))
                        continue
                    b = nb()
                    for c in range(KD):
                        mm(ps[b][:, :n], wv[:, c, :], Hv[:, c, :n], c == 0, c == KD - 1, (('W', k), 'H'), (('ps', b),), c == KD - 1)
                    if jU <= j < jGV:
                        gelu_tanh(UTv[:, j - jU, :n], ps[b][:, :n], n, (('ps', b),), ('UT',))
                        continue
                    ov, ok_ = ostage()
                    if j < jK:
                        act(ov[:, :n], ps[b][:, :n], AF.Copy, (('ps', b),), (ok_,), scale=128.0 ** -0.5)
                        dst = QT[j * 128:(j + 1) * 128, t0:t0 + n]; dk = 'QT'
                    elif j < jV:
                        psum_evac_copy(j, ov[:, :n], ps[b][:, :n], (('ps', b),), (ok_,))
                        dst = KT[(j - jK) * 128:(j - jK + 1) * 128, t0:t0 + n]; dk = 'KT'
                    elif j < jU:
                        psum_evac_copy(j, ov[:, :n], ps[b][:, :n], (('ps', b),), (ok_,))
                        dst = VT[(j - jV) * 128:(j - jV + 1) * 128, t0:t0 + n]; dk = 'VT'
                    elif j < jGB:
                        act(ov[:, :n], ps[b][:, :n], AF.Sigmoid, (('ps', b),), (ok_,))
                        dst = SGA[(j - jGA) * 128:(j - jGA + 1) * 128, t0:t0 + n]; dk = 'SGA'
                    else:
                        act(ov[:, :n], ps[b][:, :n], AF.Sigmoid, (('ps', b),), (ok_,))
                        dst = SGB[(j - jGB) * 128:(j - jGB + 1) * 128, t0:t0 + n]; dk = 'SGB'
                    dma(dst, ov[:, :n], (ok_,), (dk,))

            ck('p2')
            fence(('WS', 'BSB', 'GNG', 'UT', 'VTMP', 'SQ', 'SD', 'TMP') + tuple(('GBUF', i) for i in range(4)) + tuple(('VTOK', i) for i in range(4)) + tuple(('XS', i) for i in range(2)) + tuple(('GSUM', i) for i in range(4)) + tuple(('OST', i) for i in range(4)) + ('KTh', 'QTh', 'VTh', 'BIA', 'Vt', 'OAh', 'S', 'E', 'P') + tuple(('PT', i) for i in range(7)))
            TB2 = T * 2
            KTh = view(oBIG, TB2, BF16); QTh = view(oBIG + TB2, TB2, BF16); VTh = view(oBIG + 2 * TB2, TB2, BF16)
            Vt = view(oBIG + 3 * TB2, TB2, BF16, "p (k d) -> p k d", d=128)
            OAh = view(oBIG + 4 * TB2, TB2, BF16)
            BIA = view(oBIG + 5 * TB2, 5 * 640 * 4, F32, "p (a k) -> p a k", a=5)
            assert 5 * TB2 + 5 * 640 * 4 <= 64 * 1024
            Sv = view(oXS, 896 * 4, F32); Ev = view(oXS + 4096, 896 * 4, F32)
            Pv = view(oXS + 8192, 896 * 2, BF16); PTv = view(oXS + 10240, 7 * 256, BF16, "p (k q) -> p k q", k=7)
            attw = ('KTh', 'QTh', 'VTh', 'BIA')
            for h in range(H):
                hs = slice(h * 128, (h + 1) * 128)
                dma(KTh, KT[hs, :], ('KT',), ('KTh',))
                dma(QTh, QT[hs, :], ('QT',), ('QTh',))
                dma(VTh, VT[hs, :], ('VT',), ('VTh',))
                dma(BIA.rearrange("p a k -> p (a k)"), biasd[l, h], (), ('BIA',))
                if cfg.get('p3mode') == 'load':
                    continue
                Vt2 = Vt.rearrange("p k d -> p (k d)")
                nvt = T // 128
                for g0 in range(0, nvt, 4):
                    gn = min(4, nvt - g0)
                    bq = nb()
                    for i in range(gn):
                        mm(ps[bq][:, i * 128:(i + 1) * 128], VTh[:, (g0 + i) * 128:(g0 + i + 1) * 128], IDB, True, True, ('VTh', 'IDB'), (('ps', bq),), i == gn - 1)
                    psum_evac_copy(g0 // 4, Vt2[:, g0 * 128:(g0 + gn) * 128], ps[bq][:, :gn * 128], (('ps', bq),), ('Vt',))
                if cfg.get('p3mode') in ('vt', 'vt_mm'):
                    continue

                def softmax_pv(ncols, qsl, vtiles):
                    if cfg.get('p3mode') == 'qk':
                        return
                    m, km = smcol(); ssum, ks = smcol()
                    dve(lambda e: e.reduce_max(m, Sv[:, :ncols], axis=AX.X), ('S',), (km,))
                    dve(lambda e: e.tensor_scalar(m, m, -1.0, None, ALU.mult), (km,), (km,))
                    act(Ev[:, :ncols], Sv[:, :ncols], AF.Exp, ('S', km), ('E', ks), bias=m, accum=ssum)
                    dve(lambda e: e.reciprocal(ssum, ssum), (ks,), (ks,))
                    dve(lambda e: e.tensor_scalar(Pv[:, :ncols], Ev[:, :ncols], ssum, None, ALU.mult), ('E', ks), ('P',))
                    nk = ncols // 128
                    if cfg.get('p3mode') == 'sm':
                        return
                    PT2 = PTv.rearrange("p k q -> p (k q)")
                    for g0 in range(0, nk, 4):
                        gn = min(4, nk - g0)
                        bq = nb()
                        for i in range(gn):
                            mm(ps[bq][:, i * 128:(i + 1) * 128], Pv[:, (g0 + i) * 128:(g0 + i + 1) * 128], IDB, True, True, ('P', 'IDB'), (('ps', bq),), i == gn - 1)
                        psum_evac_copy(g0 // 4, PT2[:, g0 * 128:(g0 + gn) * 128], ps[bq][:, :gn * 128], (('ps', bq),), tuple(('PT', g0 + i) for i in range(gn)))
                    b = nb()
                    for k2 in range(nk):
                        mm(ps[b][:, :128], Vt[:, vtiles[k2], :], PTv[:, k2, :], k2 == 0, k2 == nk - 1, ('Vt', ('PT', k2)), (('ps', b),), k2 == nk - 1)
                    act(OAh[:, qsl], ps[b][:, :128], AF.Copy, (('ps', b),), ('OAh',))

                for r0 in range(0, rows, 2):
                    be = min(max(r0 - 4, 0), rows - 10)
                    pat = 0 if r0 == 0 else 1 if r0 == 2 else 3 if r0 == rows - 4 else 4 if r0 == rows - 2 else 2
                    qsl = slice(64 * r0, 64 * r0 + 128)
                    k0 = 64 * be
                    mm(ps[5][:, :512], QTh[:, qsl], KTh[:, k0:k0 + 512], True, True, ('QTh', 'KTh'), (('ps', 5),), True)
                    mm(ps[0][:, 0:128], QTh[:, qsl], KTh[:, k0 + 512:k0 + 640], True, True, ('QTh', 'KTh'), (('ps', 0),), False)
                    mm(ps[0][:, 128:384], QTh[:, qsl], KTh[:, N:N + C], True, True, ('QTh', 'KTh'), (('ps', 0),), True)
                    bank[0] = 0
                    dve(lambda e, pat=pat: e.tensor_tensor(Sv[:, 0:512], ps[5][:, :512], BIA[:, pat, 0:512], ALU.add), (('ps', 5), 'BIA'), ('S',))
                    dve(lambda e, pat=pat: e.tensor_tensor(Sv[:, 512:640], ps[0][:, 0:128], BIA[:, pat, 512:640], ALU.add), (('ps', 0), 'BIA', 'S'), ('S',))
                    act(Sv[:, 640:896], ps[0][:, 128:384], AF.Copy, (('ps', 0), 'S'), ('S',))
                    softmax_pv(896, qsl, [k0 // 128 + i for i in range(5)] + [N // 128, N // 128 + 1])
                if not last:
                    for tq in range(C // 128):
                        qsl = slice(N + tq * 128, N + (tq + 1) * 128)
                        mm(ps[0][:, 0:C], QTh[:, qsl], KTh[:, N:N + C], True, True, ('QTh', 'KTh'), (('ps', 0),), True)
                        bank[0] = 0
                        act(Sv[:, 0:C], ps[0][:, 0:C], AF.Copy, (('ps', 0),), ('S',))
                        softmax_pv(C, qsl, [N // 128 + i for i in range(C // 128)])
                TO = N if last else T
                dma(OAT[hs, :TO], OAh[:, :TO], ('OAh',), ('OAT',))

            ck('p3')
            fence(('KTh', 'QTh', 'VTh', 'BIA', 'Vt', 'OAh', 'S', 'E', 'P') + tuple(('PT', i) for i in range(7)) + ('XB', 'OAb', 'OBb', 'AF', 'EX', 'AFT') + ('WS', 'BSB', 'GNG', 'UT', 'VTMP', 'SQ', 'SD', 'TMP') + tuple(('GBUF', i) for i in range(4)) + tuple(('VTOK', i) for i in range(4)) + tuple(('XS', i) for i in range(2)) + tuple(('GSUM', i) for i in range(4)) + tuple(('OST', i) for i in range(4)))
            XB = view(oBIG, KD * NB * 4, F32, "p (c n) -> p c n", c=KD)
            OAb = view(oXS, KA * NB * 2, BF16, "p (c n) -> p c n", c=KA)
            OBb = view(oXS + 16 * 1024, G * NB * 2, BF16, "p (c n) -> p c n", c=G)
            assert KA * NB * 2 <= 16 * 1024 and G * NB * 2 <= 16 * 1024
            SGt = [view(oM + 8192 + i * 1024, NB * 2, BF16) for i in range(2)]
            T1 = TMPv; T2 = SDv
            AFv = view(oM + 8192 + 2048, 64 * 4, F32)[:, :E]; EXv = view(oM + 8192 + 2048 + 512, 64 * 4, F32)[:, :E]
            AFT = view(oM + 8192 + 3072, 128 * 4, F32)
            pblocks = [b_ for b_ in blocks if not (last and b_[0] >= N)]
            for bi, (t0, n) in enumerate(pblocks):
                isctx = t0 >= N
                tc_ = 1 if isctx else 0
                nt4 = n // 128
                dma(OAb[:, :, :n], chunked(OAT)[:, :, t0:t0 + n], ('OAT',), ('OAb',))
                dma(OBb[:, :, :n], chunked(OBT)[:, :, t0:t0 + n], ('OBT',), ('OBb',))
                for q in range(KD // 4):
                    dma(XB[:, q * 4:q * 4 + 4, :n], chunked(xsrc)[:, q * 4:q * 4 + 4, t0:t0 + n], ('xs',), ('XB',))
                for j in range(KD):
                    ka, wav = wload(wa[l, j], KA * 256, "p (c m) -> p c m", c=KA)
                    kb, wbv = wload(wb[l, j], G * 256, "p (c m) -> p c m", c=G)
                    dma(SGt[0][:, :n], SGA[j * 128:(j + 1) * 128, t0:t0 + n], ('SGA',), (('OST', 0),))
                    dma(SGt[1][:, :n], SGB[j * 128:(j + 1) * 128, t0:t0 + n], ('SGB',), (('OST', 1),))
                    b1 = nb()
                    for c in range(KA):
                        mm(ps[b1][:, :n], wav[:, c, :], OAb[:, c, :n], c == 0, c == KA - 1, (('W', ka), 'OAb'), (('ps', b1),), c == KA - 1)
                    b2 = nb()
                    for c in range(G):
                        mm(ps[b2][:, :n], wbv[:, c, :], OBb[:, c, :n], c == 0, c == G - 1, (('W', kb), 'OBb'), (('ps', b2),), c == G - 1)
                    dve(lambda e, b1=b1, n=n: e.tensor_tensor(T1[:, :n], ps[b1][:, :n], SGt[0][:, :n], ALU.mult), (('ps', b1), ('OST', 0)), ('TMP',))
                    dve(lambda e, b2=b2, n=n: e.tensor_tensor(T2[:, :n], ps[b2][:, :n], SGt[1][:, :n], ALU.mult), (('ps', b2), ('OST', 1)), ('SD',))
                    dve(lambda e, j=j, n=n: e.tensor_tensor(Hv[:, j, :n], T1[:, :n], T2[:, :n], ALU.add), ('TMP', 'SD'), ('H',))
                for j in range(KD):
                    k, wv = wload(wo[l, j], KD * 256, "p (c m) -> p c m", c=KD)
                    b = nb()
                    for c in range(KD):
                        mm(ps[b][:, :n], wv[:, c, :], Hv[:, c, :n], c == 0, c == KD - 1, (('W', k), 'H'), (('ps', b),), c == KD - 1)
                    dve(lambda e, b=b, j=j, n=n, tc_=tc_, MOD=MOD: e.scalar_tensor_tensor(XB[:, j, :n], ps[b][:, :n], MOD[:, 2 * KD + j, tc_:tc_ + 1], XB[:, j, :n], ALU.mult, ALU.add),
                        (('ps', b), 'XB') + mk, ('XB',))
                for q in range(KD // 4):
                    dma(chunked(xs)[:, q * 4:q * 4 + 4, t0:t0 + n], XB[:, q * 4:q * 4 + 4, :n], ('XB',), ('xs',))
                rms_block(lambda q, pas, n=n: (XB[:, q * 4:q * 4 + 4, :n], ('XB',)), n,
                          lambda c: (A2[:, c, tc_:tc_ + 1], ('A2',)), lambda c: (MOD[:, 3 * KD + c, tc_:tc_ + 1], mk),
                          lambda c: (Hv[:, c, :n], ('H',)), SQv, SDv, TMPv)
                for q in range(KD // 4):
                    dma(chunked(H2T)[:, q * 4:q * 4 + 4, t0:t0 + n], Hv[:, q * 4:q * 4 + 4, :n], ('H',), ('H2T',))
                for t4 in range(nt4):
                    for c in range(KD):
                        mm(ps[6][:, :E], Hv[:, c, t4 * 128:(t4 + 1) * 128], RWv[:, c, :], c == 0, c == KD - 1, ('H', 'RW'), (('ps', 6),), c == KD - 1)
                    m, km = smcol(); ssum, ks = smcol()
                    dve(lambda e, m=m: e.reduce_max(m, ps[6][:, :E], axis=AX.X), (('ps', 6),), (km,))
                    dve(lambda e, m=m: e.tensor_scalar(m, m, -1.0, None, ALU.mult), (km,), (km,))
                    act(EXv, ps[6][:, :E], AF.Exp, (('ps', 6), km), ('EX', ks), bias=m, accum=ssum)
                    dve(lambda e, ssum=ssum: e.reciprocal(ssum, ssum), (ks,), (ks,))
                    dve(lambda e, ssum=ssum: e.tensor_scalar(AFv, EXv, ssum, None, ALU.mult), ('EX', ks), ('AF',))
                    S.op('pe', lambda e: e.matmul(ps[6][:E, :128], AFv, IDF, start=True, stop=True), r=('AF', 'IDF'), w=(('ps', 6),))
                    act(AFT[:E, :], ps[6][:E, :128], AF.Copy, (('ps', 6),), ('AFT',))
                    dma(AFFT[:, t0 + t4 * 128:t0 + (t4 + 1) * 128], AFT[:E, :], ('AFT',), ('AFFT',))

            ck('p7')
            VV = view(oBIG, N * 4, F32, p0=0, p1=64); JK = view(oBIG + N * 4, N * 4, F32, p0=0, p1=64)
            assert 2 * N * 4 <= 64 * 1024
            fence(('XB', 'OAb', 'OBb', 'AF', 'EX', 'AFT') + ('VV', 'JK'))
            dve(lambda e: e.memset(VV, -1.0), (), ('VV',))
            dma(VV[0:E, :], AFFT[:, 0:N], ('AFFT',), ('VV',))
            if not last:
                dma(VV[32:32 + E, 0:C], AFFT[:, N:T], ('AFFT',), ('VV',))
            KK = SM[0:64, 56:57]; LO = SM[0:64, 57:58]; HI = SM[0:64, 58:59]; MID = SM[0:64, 59:60]
            CNT = SM[0:64, 60:61]; GE = SM[0:64, 61:62]; DD = SM[0:64, 62:63]
            dve(lambda e: e.memset(SM[0:32, 56:57], float(cap)), (), ('KK',))
            dve(lambda e: e.memset(SM[32:64, 56:57], float(capc)), ('KK',), ('KK',))
            dve(lambda e: e.memset(LO, 0.0), (), ('LO',))
            dve(lambda e: e.memset(HI, 1.0), (), ('HI',))
            for it in range(NITER):
                dve(lambda e: e.tensor_scalar(MID, LO, HI, 0.5, ALU.add, ALU.mult), ('LO', 'HI'), ('MID',))
                dve(lambda e: e.tensor_scalar(JK, VV, MID, None, ALU.is_ge, ALU.add, accum_out=CNT), ('VV', 'MID'), ('JK', 'CNT'))
                dve(lambda e: e.tensor_tensor(GE, CNT, KK, ALU.is_ge), ('CNT', 'KK'), ('GE',))
                dve(lambda e: e.tensor_tensor(DD, MID, LO, ALU.subtract), ('MID', 'LO'), ('DD',))
                dve(lambda e: e.scalar_tensor_tensor(LO, DD, GE, LO, ALU.mult, ALU.add), ('DD', 'GE', 'LO'), ('LO',))
                dve(lambda e: e.tensor_tensor(DD, HI, MID, ALU.subtract), ('MID', 'HI'), ('DD',))
                dve(lambda e: e.scalar_tensor_tensor(HI, DD, GE, MID, ALU.mult, ALU.add), ('DD', 'GE', 'MID'), ('HI',))
            dve(lambda e: e.scalar_tensor_tensor(JK[0:E, :], VV[0:E, :], LO[0:E, :], VV[0:E, :], ALU.is_ge, ALU.mult), ('VV', 'LO'), ('JK',))
            dma(GMT[:, 0:N], JK[0:E, :], ('JK',), ('GMT',))
            if not last:
                dve(lambda e: e.scalar_tensor_tensor(JK[32:32 + E, 0:C], VV[32:32 + E, 0:C], LO[32:32 + E, :], VV[32:32 + E, 0:C], ALU.is_ge, ALU.mult),
                    ('VV', 'LO'), ('JK',))
                dma(GMT[:, N:T], JK[32:32 + E, 0:C], ('JK',), ('GMT',))

            ck('thr')
            ACC = view(oBIG, KD * NB * 4, F32, "p (c n) -> p c n", c=KD)
            HID = view(oM, KF * NB * 2, BF16, "p (f n) -> p f n", f=KF)
            assert KF * NB * 2 <= 8192
            SAv = view(oXS, NB * 4, F32); TTv = view(oXS + 2048, NB * 4, F32); GBv = view(oXS + 4096, NB * 4, F32)
            GMv = view(oXS + 6144, NB * 4, F32)
            SELv = view(oXS + 8192, E * 512, F32, "p (e m) -> p e m", e=E)
            XP = [view(oXS + 16 * 1024 + i * 8192, 4 * NB * 4, F32, "p (c n) -> p c n", c=4) for i in range(2)]
            SQ8 = view(oM + 8192, 4 * NB * 2, BF16, "p (c n) -> p c n", c=4)
            fence(('XB', 'OAb', 'OBb', 'AF', 'EX', 'AFT') + ('VV', 'JK') + ('ACC', 'SEL', 'SA', 'TT', 'GB', 'GM', 'HID', 'SQ8', ('XP', 0), ('XP', 1)) + ('WS', 'BSB', 'GNG', 'UT', 'VTMP', 'SQ', 'SD', 'TMP') + tuple(('GBUF', i) for i in range(4)) + tuple(('VTOK', i) for i in range(4)) + tuple(('XS', i) for i in range(2)) + tuple(('GSUM', i) for i in range(4)) + tuple(('OST', i) for i in range(4)))
            dma(SELv.rearrange("p e m -> p (e m)")[0:64, :], seld, (), ('SEL',))
            for bi, (t0, n) in enumerate(pblocks):
                isctx = t0 >= N
                tc_ = 1 if isctx else 0
                for q in range(KD // 4):
                    dma(Hv[:, q * 4:q * 4 + 4, :n], chunked(H2T)[:, q * 4:q * 4 + 4, t0:t0 + n], ('H2T',), ('H',))
                dma(GMv[0:E, :n], GMT[:, t0:t0 + n], ('GMT',), ('GM',))
                for ex in range(E):
                    mm(ps[6][:, :n], SELv[0:E, ex, :], GMv[0:E, :n], True, True, ('SEL', 'GM'), (('ps', 6),), True)
                    act(GBv[:, :n], ps[6][:, :n], AF.Copy, (('ps', 6),), ('GB',))
                    for ft in range(KF):
                        k1, w1v = wload(w1[l, ex, ft], KD * 256, "p (c m) -> p c m", c=KD)
                        k3, w3v = wload(w3[l, ex, ft], KD * 256, "p (c m) -> p c m", c=KD)
                        b1 = nb()
                        for c in range(KD):
                            mm(ps[b1][:, :n], w1v[:, c, :], Hv[:, c, :n], c == 0, c == KD - 1, (('W', k1), 'H'), (('ps', b1),), c == KD - 1)
                        b3 = nb()
                        for c in range(KD):
                            mm(ps[b3][:, :n], w3v[:, c, :], Hv[:, c, :n], c == 0, c == KD - 1, (('W', k3), 'H'), (('ps', b3),), c == KD - 1)
                        act(SAv[:, :n], ps[b1][:, :n], AF.Silu, (('ps', b1),), ('SA',))
                        dve(lambda e, b3=b3, n=n: e.tensor_tensor(TTv[:, :n], SAv[:, :n], ps[b3][:, :n], ALU.mult), ('SA', ('ps', b3)), ('TT',))
                        dve(lambda e, ft=ft, n=n: e.tensor_tensor(HID[:, ft, :n], TTv[:, :n], GBv[:, :n], ALU.mult), ('TT', 'GB'), ('HID',))
                    for j in range(KD):
                        k, wv = wload(w2[l, ex, j], KF * 256, "p (c m) -> p c m", c=KF)
                        b = nb()
                        for ft in range(KF):
                            mm(ps[b][:, :n], wv[:, ft, :], HID[:, ft, :n], ft == 0, ft == KF - 1, (('W', k), 'HID'), (('ps', b),), ft == KF - 1)
                        if ex == 0:
                            act(ACC[:, j, :n], ps[b][:, :n], AF.Copy, (('ps', b),), ('ACC',))
                        else:
                            dve(lambda e, b=b, j=j, n=n: e.tensor_tensor(ACC[:, j, :n], ACC[:, j, :n], ps[b][:, :n], ALU.add), (('ps', b), 'ACC'), ('ACC',))
                for q in range(KD // 4):
                    kx = q % 2
                    dma(XP[kx][:, :, :n], chunked(xs)[:, q * 4:q * 4 + 4, t0:t0 + n], ('xs',), (('XP', kx),))
                    for i in range(4):
                        c = q * 4 + i
                        dve(lambda e, c=c, i=i, kx=kx, n=n, tc_=tc_, MOD=MOD: e.scalar_tensor_tensor(ACC[:, c, :n], ACC[:, c, :n], MOD[:, 5 * KD + c, tc_:tc_ + 1], XP[kx][:, i, :n], ALU.mult, ALU.add),
                            ('ACC', ('XP', kx)) + mk, ('ACC',))
                if not last:
                    for q in range(KD // 4):
                        dma(chunked(xs)[:, q * 4:q * 4 + 4, t0:t0 + n], ACC[:, q * 4:q * 4 + 4, :n], ('ACC',), ('xs',))
                else:
                    rms_block(lambda q, pas, n=n: (ACC[:, q * 4:q * 4 + 4, :n], ('ACC',)), n,
                              lambda c: (GFv[:, c:c + 1], ('GF',)), None,
                              lambda c: (ACC[:, c, :n], ('ACC',)), SQ8, SAv, None, sqk='SQ8', sdk='SA')
                    for q in range(KD // 4):
                        dma(chunked(yT)[:, q * 4:q * 4 + 4, t0:t0 + n], ACC[:, q * 4:q * 4 + 4, :n], ('ACC',), ('yT',))


    except _Stop:
        pass

    S.op('dve', lambda e: e.memset(SM[:, 54:55], 0.0), r=(), w=tuple(S.lastw.keys()) + ('ENDF',))
    S.op('pe', lambda e: e.matmul(ps[6][:, 0:2], ONES, ONES[:, 0:2], start=True, stop=True), r=('ENDF',), w=('ENDP',))
    S.op('sp', lambda e: e.nop(), r=('ENDF', 'ENDP'), w=('END',))
    per = S.emit(nc, es)

    with nc.Block() as block:
        def mk_body(name):
            def body(e):
                for ws, fn, so in per[name]:
                    for (s, v) in ws:
                        e.wait_ge(s, v)
                    ins = fn(e)
                    if so is not None:
                        ins.then_inc(so[0], so[2])
            return body
        block.tensor(mk_body('pe'))
        block.scalar(mk_body('act'))
        block.vector(mk_body('dve'))
        block.gpsimd(mk_body('pool'))
        block.sync(mk_body('sp'))
    es.close()
    return nc


def tile_w(W):
    K, M = W.shape
    return np.ascontiguousarray(W.reshape(K // 128, 128, M // 128, 128).transpose(2, 1, 0, 3)).reshape(M // 128, 128, (K // 128) * 128)


def bias_tables(rpb, rows):
    Hh = rpb.shape[0]
    out = np.full((Hh, 128, 5, 640), NEG, np.float32)
    c = np.arange(64)
    cs = np.clip(c - 8, 0, 48)
    kc = np.arange(64)
    for pi, r0 in enumerate([0, 2, 4, rows - 4, rows - 2]):
        be = min(max(r0 - 4, 0), rows - 10)
        for rr in range(2):
            r = r0 + rr
            rs = min(max(r - 4, 0), rows - 8)
            for i in range(10):
                kr = be + i
                if not (rs <= kr <= rs + 7):
                    continue
                valid = (kc[None, :] >= cs[:, None]) & (kc[None, :] < cs[:, None] + 16)
                coff = np.clip(kc[None, :] - c[:, None] + 15, 0, 30)
                vals = rpb[:, kr - r + 7, :][:, coff]
                blk = np.where(valid[None], vals, NEG)
                out[:, rr * 64:(rr + 1) * 64, pi, i * 64:(i + 1) * 64] = blk
    return out.reshape(Hh, 128, 5 * 640)


def prep_inputs(cfg, inp, s):
    D, N, C, H, GW, E, FF, L = (cfg[k] for k in ('D', 'N', 'C', 'H', 'GW', 'E', 'FF', 'L'))
    KD, G = D // 128, GW // 128
    f = lambda a: np.ascontiguousarray(np.asarray(a, np.float32))
    pc = lambda v: f(np.asarray(v).reshape(KD, 128).T)
    m = {}
    m["xT"] = f(np.concatenate([inp["x"][s].T, inp["ctx"][s].T], axis=1))
    m["cT"] = f(np.stack([pc(inp["c"][s]), pc(inp["c_ctx"])], axis=2).reshape(128, KD * 2))
    m["adaw"] = f(np.stack([tile_w(inp["ada_w"][l]) for l in range(L)]))
    m["adab"] = f(np.stack([inp["ada_b"][l].reshape(6 * KD, 128).T for l in range(L)]))
    m["g1"] = f(np.stack([pc(inp["norm1_g"][l]) for l in range(L)]))
    m["g2"] = f(np.stack([pc(inp["norm2_g"][l]) for l in range(L)]))
    m["gf"] = pc(inp["final_norm_g"])
    m["win"] = f(np.stack([tile_w(inp["w_in"][l]) for l in range(L)]))
    m["bias"] = f(np.stack([bias_tables(np.asarray(inp["na_rpb"][l]), N // 64) for l in range(L)]))
    m["gng"] = f(np.asarray(inp["gmlp_norm_g"]).reshape(L, 1, GW))
    m["wsT"] = f(np.stack([np.asarray(inp["gmlp_ws"][l]).transpose(2, 0, 1).reshape(128, G * 128) for l in range(L)]))
    m["gbs"] = f(np.asarray(inp["gmlp_bs"]).reshape(L, 1, G * 128))
    m["wa"] = f(np.stack([tile_w(inp["w_branch_a"][l]) for l in range(L)]))
    m["wb"] = f(np.stack([tile_w(inp["w_branch_b"][l]) for l in range(L)]))
    m["wo"] = f(np.stack([tile_w(inp["w_out"][l]) for l in range(L)]))
    m["rw"] = f(np.stack([np.asarray(inp["router_w"][l]).reshape(KD, 128, E).transpose(1, 0, 2).reshape(128, KD * E) for l in range(L)]))
    m["w1"] = f(np.stack([np.stack([tile_w(inp["exp_w1"][l][e]) for e in range(E)]) for l in range(L)]))
    m["w3"] = f(np.stack([np.stack([tile_w(inp["exp_w3"][l][e]) for e in range(E)]) for l in range(L)]))
    m["w2"] = f(np.stack([np.stack([tile_w(inp["exp_w2"][l][e]) for e in range(E)]) for l in range(L)]))
    sel = np.zeros((64, E, 128), np.float32)
    for e in range(E):
        sel[e, e, :] = 1.0
        sel[32 + e, e, :] = 1.0
    m["sel"] = sel.reshape(64, E * 128)
    m["ident"] = np.eye(128, dtype=np.float32)
    return m


def run(cfg, inp, cores=None):
    B = np.asarray(inp["x"]).shape[0]
    inp = {k: np.asarray(v) for k, v in inp.items()}
    nc = build(cfg)
    in_maps = [prep_inputs(cfg, inp, 0)]
    for s in range(1, B):
        ms = dict(in_maps[0])
        D_, KD_ = cfg['D'], cfg['D'] // 128
        pc = lambda v: np.ascontiguousarray(np.asarray(v, np.float32).reshape(KD_, 128).T)
        ms["xT"] = np.ascontiguousarray(np.concatenate([inp["x"][s].T, inp["ctx"][s].T], axis=1).astype(np.float32))
        ms["cT"] = np.ascontiguousarray(np.stack([pc(inp["c"][s]), pc(inp["c_ctx"])], axis=2).reshape(128, KD_ * 2))
        in_maps.append(ms)
    res = run_bass_kernel_spmd(nc, in_maps, core_ids=list(range(B)))
    out = np.stack([np.ascontiguousarray(res.results[s]["yT"].T) for s in range(B)], axis=0)
    return out.astype(np.float32)


def kernel(**inputs):
    return run(FULL, inputs)
```
